# Optimizing a Trainium2 kernel written in Bass

```python
import math
import jax, jax.numpy as jnp
from jax import lax
import numpy as np

D_MODEL = 2048
BATCH = 16
SEQ = 2048
DEPTH = 1

ATTN_WIDTH = D_MODEL // 2
SSM_WIDTH = D_MODEL - ATTN_WIDTH
N_HEADS = 16
HEAD_DIM = ATTN_WIDTH // N_HEADS
MOBA_BLOCK = 256
MOBA_TOPK = 3
Q_CHUNK = 8
SSM_GROUP = 16
SSM_GROUPS = SSM_WIDTH // SSM_GROUP
SSM_STATE = 64
D_FF = 4 * D_MODEL
PROJ_WIDTH = 3 * ATTN_WIDTH + SSM_WIDTH
EPS = 1e-6
DT_MIN = 1e-3
DT_MAX = 1e-1
NEG = -1e30

kernel_name = "hymba_moba_s5_hybrid_block"


def rms_norm_f32(x, g):
    xf = x.astype(jnp.float32)
    y = xf * lax.rsqrt(jnp.mean(xf * xf, axis=-1, keepdims=True) + EPS)
    return y * g.astype(jnp.float32)


def rms_norm(x, g):
    return rms_norm_f32(x, g).astype(x.dtype)


def alibi_slopes(n_heads):
    return jnp.exp2(-8.0 * (jnp.arange(n_heads, dtype=jnp.float32) + 1.0) / n_heads)


def moba_attention(q, k, v):
    B_, H, L, Dh = q.shape
    nb = -(-L // MOBA_BLOCK)
    pad = nb * MOBA_BLOCK - L
    k_blk = jnp.pad(k, ((0, 0), (0, 0), (0, pad), (0, 0))).reshape(B_, H, nb, MOBA_BLOCK, Dh)
    v_blk = jnp.pad(v, ((0, 0), (0, 0), (0, pad), (0, 0))).reshape(B_, H, nb, MOBA_BLOCK, Dh)
    k_mean = jnp.mean(k_blk, axis=3)
    top_k = min(MOBA_TOPK, nb - 1)
    slopes = alibi_slopes(H)[None, :, None, None]
    scale = Dh ** -0.5
    n_chunks = L // Q_CHUNK
    q_c = q.reshape(B_, H, n_chunks, Q_CHUNK, Dh).transpose(2, 0, 1, 3, 4)
    b_idx = jnp.arange(B_)[:, None, None, None]
    h_idx = jnp.arange(H)[None, :, None, None]
    blk_pos = jnp.arange(MOBA_BLOCK)
    blk_ids = jnp.arange(nb)

    def chunk_fn(args):
        qc, c = args
        t = c * Q_CHUNK + jnp.arange(Q_CHUNK)
        own = (c * Q_CHUNK) // MOBA_BLOCK
        k_own = lax.dynamic_index_in_dim(k_blk, own, axis=2, keepdims=False)
        v_own = lax.dynamic_index_in_dim(v_blk, own, axis=2, keepdims=False)
        s_own = own * MOBA_BLOCK + blk_pos
        dist_own = (t[:, None] - s_own[None, :]).astype(jnp.float32)
        logit_own = jnp.einsum('bhqd,bhkd->bhqk', qc, k_own) * scale - slopes * dist_own
        logit_own = jnp.where(s_own[None, :] <= t[:, None], logit_own, NEG)
        if top_k == 0:
            p = jax.nn.softmax(logit_own, axis=-1)
            return jnp.einsum('bhqk,bhkd->bhqd', p, v_own)
        gate = jnp.einsum('bhqd,bhnd->bhqn', qc, k_mean)
        gate = jnp.where(blk_ids < own, gate, NEG)
        _, sel = lax.top_k(gate, top_k)
        valid = jnp.arange(top_k) < jnp.minimum(own, top_k)
        k_sel = k_blk[b_idx, h_idx, sel]
        v_sel = v_blk[b_idx, h_idx, sel]
        s_pos = sel[..., None] * MOBA_BLOCK + blk_pos
        dist_past = (t[:, None, None] - s_pos).astype(jnp.float32)
        logit_past = jnp.einsum('bhqd,bhqjkd->bhqjk', qc, k_sel) * scale - slopes[..., None] * dist_past
        logit_past = jnp.where(valid[:, None], logit_past, NEG)
        n_past = top_k * MOBA_BLOCK
        logits = jnp.concatenate([logit_past.reshape(B_, H, Q_CHUNK, n_past), logit_own], axis=-1)
        p = jax.nn.softmax(logits, axis=-1)
        p_past = p[..., :n_past].reshape(B_, H, Q_CHUNK, top_k, MOBA_BLOCK)
        return (jnp.einsum('bhqjk,bhqjkd->bhqd', p_past, v_sel)
                + jnp.einsum('bhqk,bhkd->bhqd', p[..., n_past:], v_own))

    out = lax.map(chunk_fn, (q_c, jnp.arange(n_chunks)))
    return out.transpose(1, 2, 0, 3, 4).reshape(B_, H, L, Dh)


def s5_ssm(u, lambda_re, lambda_im, log_dt, b_re, b_im, c_re, c_im, d_skip):
    B_, L, _ = u.shape
    f32 = jnp.float32
    uf = u.astype(f32).reshape(B_, L, SSM_GROUPS, SSM_GROUP)
    dt = jnp.exp(log_dt.astype(f32))[:, None]
    lr = lambda_re.astype(f32)
    li = lambda_im.astype(f32)
    mag = jnp.exp(lr * dt)
    ab_re = mag * jnp.cos(li * dt)
    ab_im = mag * jnp.sin(li * dt)
    den = lr * lr + li * li
    nr = ab_re - 1.0
    f_re = (nr * lr + ab_im * li) / den
    f_im = (ab_im * lr - nr * li) / den
    br = b_re.astype(f32)
    bi = b_im.astype(f32)
    bb_re = f_re[..., None] * br - f_im[..., None] * bi
    bb_im = f_re[..., None] * bi + f_im[..., None] * br
    bu_re = jnp.einsum('blgh,gph->lbgp', uf, bb_re)
    bu_im = jnp.einsum('blgh,gph->lbgp', uf, bb_im)
    a_re = jnp.broadcast_to(ab_re, (L, 1, SSM_GROUPS, SSM_STATE))
    a_im = jnp.broadcast_to(ab_im, (L, 1, SSM_GROUPS, SSM_STATE))

    def combine(e_i, e_j):
        air, aii, bir, bii = e_i
        ajr, aji, bjr, bji = e_j
        return (ajr * air - aji * aii,
                ajr * aii + aji * air,
                ajr * bir - aji * bii + bjr,
                ajr * bii + aji * bir + bji)

    _, _, s_re, s_im = lax.associative_scan(combine, (a_re, a_im, bu_re, bu_im), axis=0)
    y = (jnp.einsum('lbgp,ghp->blgh', s_re, c_re.astype(f32))
         - jnp.einsum('lbgp,ghp->blgh', s_im, c_im.astype(f32)))
    y = y + d_skip.astype(f32).reshape(SSM_GROUPS, SSM_GROUP) * uf
    return y.reshape(B_, L, SSM_WIDTH)


def setup_inputs(seed: int = 0) -> dict:
    key = jax.random.key(seed)
    ks = jax.random.split(key, 24)
    f32 = jnp.float32
    nrm = lambda k, shape, s: jax.random.normal(k, shape, f32) * s
    gain = lambda k, shape: 1.0 + 0.02 * jax.random.normal(k, shape, f32)
    n_idx = jnp.arange(SSM_STATE, dtype=f32)
    return {
        "x": jax.random.normal(ks[0], (BATCH, SEQ, D_MODEL), f32),
        "norm1_gain": gain(ks[1], (DEPTH, D_MODEL)),
        "w_in": nrm(ks[2], (DEPTH, D_MODEL, PROJ_WIDTH), D_MODEL ** -0.5),
        "q_norm_gain": gain(ks[3], (DEPTH, HEAD_DIM)),
        "k_norm_gain": gain(ks[4], (DEPTH, HEAD_DIM)),
        "attn_out_gain": gain(ks[5], (DEPTH, ATTN_WIDTH)),
        "lambda_re": -0.5 + 0.01 * jax.random.normal(ks[6], (DEPTH, SSM_GROUPS, SSM_STATE), f32),
        "lambda_im": math.pi * n_idx + 0.01 * jax.random.normal(ks[7], (DEPTH, SSM_GROUPS, SSM_STATE), f32),
        "log_dt": jax.random.uniform(ks[8], (DEPTH, SSM_GROUPS), f32, math.log(DT_MIN), math.log(DT_MAX)),
        "b_re": nrm(ks[9], (DEPTH, SSM_GROUPS, SSM_STATE, SSM_GROUP), (2.0 * SSM_GROUP) ** -0.5),
        "b_im": nrm(ks[10], (DEPTH, SSM_GROUPS, SSM_STATE, SSM_GROUP), (2.0 * SSM_GROUP) ** -0.5),
        "c_re": nrm(ks[11], (DEPTH, SSM_GROUPS, SSM_GROUP, SSM_STATE), (2.0 * SSM_STATE) ** -0.5),
        "c_im": nrm(ks[12], (DEPTH, SSM_GROUPS, SSM_GROUP, SSM_STATE), (2.0 * SSM_STATE) ** -0.5),
        "d_skip": nrm(ks[13], (DEPTH, SSM_WIDTH), 1.0),
        "w_glu": nrm(ks[14], (DEPTH, SSM_WIDTH, SSM_WIDTH), SSM_WIDTH ** -0.5),
        "b_glu": nrm(ks[15], (DEPTH, SSM_WIDTH), 0.02),
        "ssm_out_gain": gain(ks[16], (DEPTH, SSM_WIDTH)),
        "w_out": nrm(ks[17], (DEPTH, D_MODEL, D_MODEL), D_MODEL ** -0.5),
        "norm2_gain": gain(ks[18], (DEPTH, D_MODEL)),
        "w_ff1": nrm(ks[19], (DEPTH, D_MODEL, D_FF), D_MODEL ** -0.5),
        "w_ff2": nrm(ks[20], (DEPTH, D_FF, D_MODEL), D_FF ** -0.5),
    }


def reference(x, norm1_gain, w_in, q_norm_gain, k_norm_gain, attn_out_gain,
              lambda_re, lambda_im, log_dt, b_re, b_im, c_re, c_im, d_skip,
              w_glu, b_glu, ssm_out_gain, w_out, norm2_gain, w_ff1, w_ff2):
    B_, L, _ = x.shape

    def to_heads(t):
        return t.reshape(B_, L, N_HEADS, HEAD_DIM).transpose(0, 2, 1, 3)

    for i in range(DEPTH):
        h = rms_norm(x, norm1_gain[i])
        proj = jnp.einsum('bld,de->ble', h, w_in[i])
        q, k, v, u = jnp.split(proj, [ATTN_WIDTH, 2 * ATTN_WIDTH, 3 * ATTN_WIDTH], axis=-1)
        qh = rms_norm_f32(to_heads(q), q_norm_gain[i])
        kh = rms_norm_f32(to_heads(k), k_norm_gain[i])
        vh = to_heads(v).astype(jnp.float32)
        attn = moba_attention(qh, kh, vh)
        attn = attn.transpose(0, 2, 1, 3).reshape(B_, L, ATTN_WIDTH)
        ssm = s5_ssm(u, lambda_re[i], lambda_im[i], log_dt[i], b_re[i], b_im[i],
                     c_re[i], c_im[i], d_skip[i])
        ssm = jax.nn.gelu(ssm)
        ssm = ssm * jax.nn.sigmoid(jnp.einsum('blc,ce->ble', ssm, w_glu[i].astype(jnp.float32))
                                   + b_glu[i].astype(jnp.float32))
        mixed = jnp.concatenate([rms_norm_f32(attn, attn_out_gain[i]),
                                 rms_norm_f32(ssm, ssm_out_gain[i])], axis=-1).astype(x.dtype)
        x = x + jnp.einsum('blc,cd->bld', mixed, w_out[i])
        h2 = rms_norm(x, norm2_gain[i])
        ff = jnp.square(jax.nn.relu(jnp.einsum('bld,df->blf', h2, w_ff1[i])))
        x = x + jnp.einsum('blf,fd->bld', ff, w_ff2[i])
    return x
```

```python
import contextlib
import math
import numpy as np
import ml_dtypes
import concourse.bass as bass
import concourse.mybir as mybir
from concourse.bass_utils import run_bass_kernel_spmd

F32 = mybir.dt.float32
BF16 = mybir.dt.bfloat16
AF = mybir.ActivationFunctionType
ALU = mybir.AluOpType

NCORES = 8
NB = 2
L = 2048
D = 2048
NT = L // 128
KT = D // 128
H = 16
DH = 64
NA = 14
EPS = 1e-6
SEM_LIM = 12000
_SKIP = set()
NEGBIG = -30000.0


class _Stop(Exception):
    pass


class Sem:
    def __init__(self, h):
        self.h = h
        self.cnt = 0


class Buf:
    __slots__ = ("w", "r")

    def __init__(self):
        self.w = {}
        self.r = {}


def _merge(d, s):
    for k, v in s.items():
        if d.get(k, 0) < v:
            d[k] = v


class KB:
    def __init__(self, nc, es):
        self.nc = nc
        self.es = es
        self.E = {"pe": nc.tensor, "act": nc.scalar, "dve": nc.vector, "pool": nc.gpsimd, "sp": nc.sync}
        self.seen = {e: {} for e in self.E}
        self.nsem = 0
        self.allsems = []
        self.pe_sems = set()
        self.esem = {}
        self.esems_all = set()
        for e in ("pe", "act", "dve", "pool"):
            self.esem[e] = self.newsem(e)
            self.esems_all.add(self.esem[e])
            if e == "pe":
                self.pe_sems.add(self.esem[e])

    def newsem(self, name):
        self.nsem += 1
        s = Sem(self.es.enter_context(self.nc.semaphore(f"{name}_{self.nsem}")))
        self.allsems.append(s)
        return s

    def wait(self, eng, deps):
        seen = self.seen[eng]
        for s, v in deps.items():
            if eng == "pe" and s in self.pe_sems:
                continue
            if s not in self.esems_all:
                v = max(v, s.cnt)
            if seen.get(s, 0) >= v:
                continue
            self.E[eng].wait_ge(s.h, v)
            seen[s] = v

    def deps(self, reads, writes):
        d = {}
        for b in reads:
            _merge(d, b.w)
        for b in writes:
            _merge(d, b.w)
            _merge(d, b.r)
        return d

    def op(self, eng, fn, reads=(), writes=(), signal=True, part=False):
        self.wait(eng, self.deps(reads, writes))
        inst = fn()
        s = self.esem[eng]
        if signal:
            s.cnt += 1
            inst.then_inc(s.h, 1)
            v = s.cnt
            if s.cnt >= SEM_LIM:
                self.esem[eng] = self.newsem(eng)
                self.esems_all.add(self.esem[eng])
                if eng == "pe":
                    self.pe_sems.add(self.esem[eng])
        else:
            v = s.cnt + 1
        for b in writes:
            if part:
                b.w[s] = max(b.w.get(s, 0), v)
            else:
                b.w = {s: v}
                b.r = {}
        for b in reads:
            b.r[s] = max(b.r.get(s, 0), v)
        return inst

    def dma(self, q, out, in_, sem, reads=(), writes=(), part=False):
        self.wait(q, self.deps(reads, writes))
        inst = self.E[q].dma_start(out=out, in_=in_)
        sem.cnt += 16
        inst.then_inc(sem.h, 16)
        v = sem.cnt
        for b in writes:
            if part:
                b.w[sem] = max(b.w.get(sem, 0), v)
            else:
                b.w = {sem: v}
                b.r = {}
        for b in reads:
            b.r[sem] = max(b.r.get(sem, 0), v)
        return inst

    def barrier(self):
        allv = {s: s.cnt for s in self.allsems if s.cnt > 0}
        for e in self.E:
            self.wait(e, allv)


def build(dbg=(), stop_after=None):
    nc = bass.Bass("TRN2", target_bir_lowering=False)
    es = contextlib.ExitStack()
    dbg_out = {}

    def din(name, shape, dt=F32):
        return nc.dram_tensor(name, list(shape), dt, kind="ExternalInput").ap()

    def dscr(name, shape, dt=BF16):
        if name in dbg:
            t = nc.dram_tensor(name, list(shape), dt, kind="ExternalOutput").ap()
            dbg_out[name] = t
            return t
        return nc.dram_tensor(name, list(shape), dt).ap()

    x = din("x", [NB, L, D])
    out = nc.dram_tensor("out", [NB, L, D], F32, kind="ExternalOutput").ap()
    w_in = din("w_in", [D, 4096])
    w_out = din("w_out", [D, D])
    w_ff1 = din("w_ff1", [D, 8192])
    w_ff2 = din("w_ff2", [8192, D])
    w_glu = din("w_glu", [1024, 1024])
    g1t = din("g1t", [128, 16])
    g2t = din("g2t", [128, 16])
    goutt = din("goutt", [128, 16])
    gqk = din("gqk", [128, 2])
    bglu = din("bglu", [128, 8])
    lam_re = din("lam_re", [128, 64])
    lam_im = din("lam_im", [128, 64])
    logdt = din("logdt", [128, 64])
    b_re = din("b_re", [128, 64, 16])
    b_im = din("b_im", [128, 64, 16])
    c_re = din("c_re", [128, 64, 16])
    c_im = din("c_im", [128, 64, 16])
    drep = din("drep", [128, 64])
    c_identf = din("c_identf", [128, 128])
    c_identb = din("c_identb", [128, 128], BF16)
    c_blk = din("c_blk", [128, 128], BF16)
    c_cmask = din("c_cmask", [128, 4, 512], BF16)
    c_kaug = din("c_kaug", [H, NA, L], BF16)
    c_qaug = din("c_qaug", [H, NA, L], BF16)
    c_padm = din("c_padm", [128, 8, 8])
    c_negm = din("c_negm", [128, 8, 8])
    c_tmask = din("c_tmask", [128, 128])

    w_in_t = dscr("w_in_t", [32, 128, KT, 128])
    w_out_t = dscr("w_out_t", [4, 128, KT, 512])
    w_ff1_t = dscr("w_ff1_t", [64, 128, KT, 128])
    w_ff2_b = dscr("w_ff2_b", [8192, D])
    w_glu_b = dscr("w_glu_b", [1024, 1024])
    wz_d = dscr("wz_d", [64, 128, 256])
    wy_d = dscr("wy_d", [64, 128, 128])
    wc_d = dscr("wc_d", [64, 128, 256])
    cs_d = dscr("cs_d", [32, 128, 512], F32)
    u_d = dscr("u_d", [1024, 2048])
    y_d = dscr("y_d", [1024, 2048])
    attn_d = dscr("attn_d", [1024, 2048])
    ssm_d = dscr("ssm_d", [1024, 2048])

    try:
      with es:
        kb = KB(nc, es)
        op, dma = kb.op, kb.dma
        V = nc.vector
        dsems = {}

        def dsem(name):
            if name not in dsems:
                dsems[name] = kb.newsem(name)
            return dsems[name]

        def sb(name, shape, dt=F32):
            return es.enter_context(nc.sbuf_tensor(name, list(shape), dt))

        def dbg_dump(name, shape, src_ap, reads, dt=F32):
            if (name not in dbg and "dbg_all" not in dbg) or name in dbg_out:
                return
            t = nc.dram_tensor(name, list(shape), dt, kind="ExternalOutput").ap()
            dbg_out[name] = t
            dma("sp", t, src_ap, dsem("dbg"), reads=reads, writes=[Buf()])

        bank = [es.enter_context(nc.psum_tensor(f"bank{i}", [128, 512], F32)) for i in range(8)]
        bank_b = [Buf() for _ in range(8)]
        PP = (0, 1)
        PST = (2, 3)
        PM = (4, 5)
        PO = 6
        PTR = 7
        po = bank[PO][:, 0:260].rearrange("p (q c) -> p q c", c=65)
        po_b = bank_b[PO]
        ptr = bank[PTR][:].bitcast(BF16).rearrange("p (t q) -> p t q", q=128)
        ptr_b = bank_b[PTR]
        rr = {"pp": 0, "pm": 0, "pst": 0}

        def nxt(kind):
            i = rr[kind]
            rr[kind] = (i + 1) % 2
            return {"pp": PP, "pm": PM, "pst": PST}[kind][i]

        cb = Buf()
        identf = sb("identf", [128, 128])
        identb = sb("identb", [128, 128], BF16)
        blk = sb("blk", [128, 128], BF16)
        cmask = sb("cmask", [128, 4, 512], BF16)
        padm = sb("padm", [128, 8, 8])
        negm = sb("negm", [128, 8, 8])
        g1s = sb("g1s", [128, 16])
        g2s = sb("g2s", [128, 16])
        gouts = sb("gouts", [128, 16])
        gqks = sb("gqks", [128, 2])
        bglus = sb("bglus", [128, 8])
        nbglus = sb("nbglus", [128, 8])
        epst = sb("epst", [128, 1])
        onesb = sb("onesb", [128, 2], BF16)
        R_pair = sb("R_pair", [128, 32])
        rp_b = Buf()
        for dst, src in ((identf, c_identf), (identb, c_identb), (blk, c_blk), (cmask, c_cmask), (padm, c_padm),
                         (negm, c_negm), (g1s, g1t), (g2s, g2t), (gouts, goutt), (gqks, gqk), (bglus, bglu)):
            dma("sp", dst[:], src, dsem("const"), writes=[cb], part=True)
        op("dve", lambda: V.memset(epst[:], EPS), writes=[cb], part=True)
        op("dve", lambda: V.memset(onesb[:], 1.0), writes=[cb], part=True)
        op("dve", lambda: V.tensor_scalar(out=gqks[:, 0:1], in0=gqks[:, 0:1], scalar1=DH ** -0.5, scalar2=None,
                                          op0=ALU.mult), reads=[cb], writes=[cb], part=True)
        op("dve", lambda: V.tensor_scalar(out=nbglus[:], in0=bglus[:], scalar1=-1.0, scalar2=None,
                                          op0=ALU.mult), reads=[cb], writes=[cb], part=True)

        CW = 1024
        wcv = [sb(f"wcv{i}", [128, CW]) for i in range(2)]
        wcv_b = [Buf() for _ in range(2)]
        wcvo = [sb(f"wcvo{i}", [128, CW], BF16) for i in range(2)]
        wcvo_b = [Buf() for _ in range(2)]
        wcnt = [0]
        w_in_bb, w_out_bb, w_ff1_bb, w_ff2_bb, w_glu_bb = Buf(), Buf(), Buf(), Buf(), Buf()

        def conv_chunk(src_ap, dst_ap, gain_ap, dstbuf):
            i = wcnt[0] % 2
            wcnt[0] += 1
            dma("sp", wcv[i][:], src_ap, dsem(f"wcv{i}"), writes=[wcv_b[i]])
            if gain_ap is None:
                eng = ("pool", "dve")[wcnt[0] % 2]
                if eng == "pool":
                    op("pool", lambda: nc.gpsimd.tensor_copy(out=wcvo[i][:], in_=wcv[i][:]), reads=[wcv_b[i]], writes=[wcvo_b[i]])
                else:
                    op("dve", lambda: V.tensor_copy(out=wcvo[i][:], in_=wcv[i][:]), reads=[wcv_b[i]], writes=[wcvo_b[i]])
            else:
                eng = ("pool", "dve", "act")[wcnt[0] % 3]
                if eng == "act":
                    op("act", lambda: nc.scalar.activation(out=wcvo[i][:], in_=wcv[i][:], func=AF.Copy, scale=gain_ap),
                       reads=[wcv_b[i], cb], writes=[wcvo_b[i]])
                elif eng == "dve":
                    op("dve", lambda: V.tensor_scalar(out=wcvo[i][:], in0=wcv[i][:], scalar1=gain_ap, scalar2=None, op0=ALU.mult),
                       reads=[wcv_b[i], cb], writes=[wcvo_b[i]])
                else:
                    op("pool", lambda: nc.gpsimd.tensor_scalar(out=wcvo[i][:], in0=wcv[i][:], scalar1=gain_ap, scalar2=1.0,
                                                               op0=ALU.mult, op1=ALU.mult),
                       reads=[wcv_b[i], cb], writes=[wcvo_b[i]])
            return i

        def conv_store(i, dst_ap, src_view, dstbuf):
            dma("sp", dst_ap, src_view, dsem(f"wst{i}"), reads=[wcvo_b[i]], writes=[dstbuf], part=True)

        def conv_w_in(kt):
            for c0 in range(0, 4096, CW):
                i = conv_chunk(w_in[kt * 128:(kt + 1) * 128, c0:c0 + CW], None, g1s[:, kt:kt + 1], w_in_bb)
                f0 = c0 // 128
                conv_store(i, w_in_t[f0:f0 + CW // 128, :, kt, :].rearrange("f p e -> p f e"),
                           wcvo[i][:].rearrange("p (f e) -> p f e", e=128), w_in_bb)

        def conv_w_out(kt):
            for c0 in range(0, 2048, CW):
                i = conv_chunk(w_out[kt * 128:(kt + 1) * 128, c0:c0 + CW], None, gouts[:, kt:kt + 1], w_out_bb)
                d0 = c0 // 512
                conv_store(i, w_out_t[d0:d0 + CW // 512, :, kt, :].rearrange("g p d -> p g d"),
                           wcvo[i][:].rearrange("p (g d) -> p g d", d=512), w_out_bb)

        def conv_w_ff1(kt, c0):
            i = conv_chunk(w_ff1[kt * 128:(kt + 1) * 128, c0:c0 + CW], None, g2s[:, kt:kt + 1], w_ff1_bb)
            f0 = c0 // 128
            conv_store(i, w_ff1_t[f0:f0 + CW // 128, :, kt, :].rearrange("f p e -> p f e"),
                       wcvo[i][:].rearrange("p (f e) -> p f e", e=128), w_ff1_bb)

        def conv_plain(src, dst, r, ncols, dstbuf):
            for c0 in range(0, ncols, CW):
                i = conv_chunk(src[r * 128:(r + 1) * 128, c0:c0 + CW], None, None, dstbuf)
                conv_store(i, dst[r * 128:(r + 1) * 128, c0:c0 + CW], wcvo[i][:], dstbuf)

        for kt in range(KT):
            conv_w_in(kt)

        def wprep_steps():
            for r in range(8):
                conv_plain(w_glu, w_glu_b, r, 1024, w_glu_bb)
            yield
            for kt in range(KT):
                conv_w_out(kt)
                if kt % 4 == 3:
                    yield
            for kt in range(KT):
                for c0 in range(0, 8192, CW):
                    conv_w_ff1(kt, c0)
                    if (c0 // CW) % 4 == 3:
                        yield
            for r in range(64):
                conv_plain(w_ff2, w_ff2_b, r, 2048, w_ff2_bb)
                if r % 2 == 1:
                    yield

        wprep = wprep_steps()

        def wprep_some(n=1):
            for _ in range(n):
                try:
                    next(wprep)
                except StopIteration:
                    return

        wz_db, wy_db, wc_db, cs_db = Buf(), Buf(), Buf(), Buf()
        with contextlib.ExitStack() as es2:
            def sb2(name, shape, dt=F32):
                return es2.enter_context(nc.sbuf_tensor(name, list(shape), dt))

            psem = dsem("prep")
            pb = Buf()
            lr = sb2("lr", [128, 64]); li = sb2("li", [128, 64]); ldt = sb2("ldt", [128, 64])
            bre = sb2("bre", [128, 64, 16]); bim = sb2("bim", [128, 64, 16])
            cre = sb2("cre", [128, 64, 16]); cim = sb2("cim", [128, 64, 16])
            dr = sb2("dr", [128, 64]); tmask = sb2("tmask", [128, 128])
            for dst, src in ((lr, lam_re), (li, lam_im), (ldt, logdt), (bre, b_re), (bim, b_im), (cre, c_re),
                             (cim, c_im), (dr, drep), (tmask, c_tmask)):
                dma("sp", dst[:], src, psem, writes=[pb], part=True)

            def T(name, shape=(128, 64)):
                return sb2(name, list(shape))

            def dv(fn):
                op("dve", fn, reads=[pb], writes=[pb], part=True)

            dt_ = T("dt_"); xx = T("xx"); ph = T("ph"); ph2 = T("ph2"); acc = T("acc")
            mag = T("mag"); cs_ = T("cs_"); sn_ = T("sn_"); t1 = T("t1"); t2 = T("t2"); t3 = T("t3")
            yy = T("yy")
            dv(lambda: V.tensor_scalar(out=yy[:], in0=ldt[:], scalar1=-1.0, scalar2=None, op0=ALU.mult))
            dv(lambda: V.tensor_scalar(out=acc[:], in0=yy[:], scalar1=1.0 / math.factorial(34), scalar2=None, op0=ALU.mult))
            for k in range(33, 0, -1):
                dv(lambda k=k: V.scalar_tensor_tensor(out=acc[:], in0=acc[:], scalar=1.0 / math.factorial(k), in1=yy[:],
                                                      op0=ALU.add, op1=ALU.mult))
            dv(lambda: V.tensor_scalar(out=t1[:], in0=acc[:], scalar1=1.0, scalar2=None, op0=ALU.add))
            dv(lambda: V.reciprocal(out=dt_[:], in_=t1[:]))
            dv(lambda: V.tensor_tensor(out=xx[:], in0=lr[:], in1=dt_[:], op=ALU.mult))
            dv(lambda: V.tensor_scalar(out=acc[:], in0=xx[:], scalar1=1.0 / math.factorial(10), scalar2=None, op0=ALU.mult))
            for k in range(9, 0, -1):
                dv(lambda k=k: V.scalar_tensor_tensor(out=acc[:], in0=acc[:], scalar=1.0 / math.factorial(k), in1=xx[:],
                                                      op0=ALU.add, op1=ALU.mult))
            dv(lambda: V.tensor_scalar(out=mag[:], in0=acc[:], scalar1=1.0, scalar2=None, op0=ALU.add))
            dv(lambda: V.scalar_tensor_tensor(out=ph[:], in0=li[:], scalar=1.0 / 16.0, in1=dt_[:], op0=ALU.mult, op1=ALU.mult))
            dv(lambda: V.tensor_tensor(out=ph2[:], in0=ph[:], in1=ph[:], op=ALU.mult))
            dv(lambda: V.tensor_scalar(out=acc[:], in0=ph2[:], scalar1=1.0 / math.factorial(20), scalar2=None, op0=ALU.mult))
            for k in range(9, 0, -1):
                dv(lambda k=k: V.scalar_tensor_tensor(out=acc[:], in0=acc[:], scalar=(-1.0) ** k / math.factorial(2 * k), in1=ph2[:],
                                                      op0=ALU.add, op1=ALU.mult))
            dv(lambda: V.tensor_scalar(out=cs_[:], in0=acc[:], scalar1=1.0, scalar2=None, op0=ALU.add))
            dv(lambda: V.tensor_scalar(out=acc[:], in0=ph2[:], scalar1=1.0 / math.factorial(21), scalar2=None, op0=ALU.mult))
            for k in range(9, 0, -1):
                dv(lambda k=k: V.scalar_tensor_tensor(out=acc[:], in0=acc[:], scalar=(-1.0) ** k / math.factorial(2 * k + 1), in1=ph2[:],
                                                      op0=ALU.add, op1=ALU.mult))
            dv(lambda: V.scalar_tensor_tensor(out=sn_[:], in0=acc[:], scalar=1.0, in1=ph[:], op0=ALU.add, op1=ALU.mult))
            for _ in range(4):
                dv(lambda: V.tensor_tensor(out=t1[:], in0=cs_[:], in1=cs_[:], op=ALU.mult))
                dv(lambda: V.tensor_tensor(out=t2[:], in0=sn_[:], in1=sn_[:], op=ALU.mult))
                dv(lambda: V.scalar_tensor_tensor(out=t3[:], in0=cs_[:], scalar=2.0, in1=sn_[:], op0=ALU.mult, op1=ALU.mult))
                dv(lambda: V.tensor_copy(out=sn_[:], in_=t3[:]))
                dv(lambda: V.tensor_tensor(out=cs_[:], in0=t1[:], in1=t2[:], op=ALU.subtract))
            Pr = T("Pr", (128, 9, 64)); Pi = T("Pi", (128, 9, 64)); Gr = T("Gr", (128, 8, 64)); Gi = T("Gi", (128, 8, 64))
            rmag = T("rmag")
            dv(lambda: V.memset(Pr[:, 0, :], 1.0)); dv(lambda: V.memset(Pi[:, 0, :], 0.0))
            dv(lambda: V.memset(Gr[:, 0, :], 1.0)); dv(lambda: V.memset(Gi[:, 0, :], 0.0))
            dv(lambda: V.tensor_tensor(out=Pr[:, 1, :], in0=mag[:], in1=cs_[:], op=ALU.mult))
            dv(lambda: V.tensor_tensor(out=Pi[:, 1, :], in0=mag[:], in1=sn_[:], op=ALU.mult))
            dv(lambda: V.reciprocal(out=rmag[:], in_=mag[:]))
            dv(lambda: V.tensor_tensor(out=Gr[:, 1, :], in0=rmag[:], in1=cs_[:], op=ALU.mult))
            dv(lambda: V.scalar_tensor_tensor(out=Gi[:, 1, :], in0=rmag[:], scalar=-1.0, in1=sn_[:], op0=ALU.mult, op1=ALU.mult))

            def cmul(o_r, o_i, a_r, a_i, b_r, b_i, ta, tb):
                dv(lambda: V.tensor_tensor(out=ta, in0=a_r, in1=b_r, op=ALU.mult))
                dv(lambda: V.tensor_tensor(out=tb, in0=a_i, in1=b_i, op=ALU.mult))
                dv(lambda: V.tensor_tensor(out=o_r, in0=ta, in1=tb, op=ALU.subtract))
                dv(lambda: V.tensor_tensor(out=ta, in0=a_r, in1=b_i, op=ALU.mult))
                dv(lambda: V.tensor_tensor(out=tb, in0=a_i, in1=b_r, op=ALU.mult))
                dv(lambda: V.tensor_tensor(out=o_i, in0=ta, in1=tb, op=ALU.add))

            for j in range(2, 9):
                cmul(Pr[:, j, :], Pi[:, j, :], Pr[:, j - 1, :], Pi[:, j - 1, :], Pr[:, 1, :], Pi[:, 1, :], t1[:], t2[:])
            for j in range(2, 8):
                cmul(Gr[:, j, :], Gi[:, j, :], Gr[:, j - 1, :], Gi[:, j - 1, :], Gr[:, 1, :], Gi[:, 1, :], t1[:], t2[:])
            den = T("den"); nr = T("nr"); fr = T("fr"); fi = T("fi")
            dv(lambda: V.tensor_tensor(out=t1[:], in0=lr[:], in1=lr[:], op=ALU.mult))
            dv(lambda: V.tensor_tensor(out=t2[:], in0=li[:], in1=li[:], op=ALU.mult))
            dv(lambda: V.tensor_tensor(out=t3[:], in0=t1[:], in1=t2[:], op=ALU.add))
            dv(lambda: V.reciprocal(out=den[:], in_=t3[:]))
            dv(lambda: V.tensor_scalar(out=nr[:], in0=Pr[:, 1, :], scalar1=-1.0, scalar2=None, op0=ALU.add))
            dv(lambda: V.tensor_tensor(out=t1[:], in0=nr[:], in1=lr[:], op=ALU.mult))
            dv(lambda: V.tensor_tensor(out=t2[:], in0=Pi[:, 1, :], in1=li[:], op=ALU.mult))
            dv(lambda: V.tensor_tensor(out=t3[:], in0=t1[:], in1=t2[:], op=ALU.add))
            dv(lambda: V.tensor_tensor(out=fr[:], in0=t3[:], in1=den[:], op=ALU.mult))
            dv(lambda: V.tensor_tensor(out=t1[:], in0=Pi[:, 1, :], in1=lr[:], op=ALU.mult))
            dv(lambda: V.tensor_tensor(out=t2[:], in0=nr[:], in1=li[:], op=ALU.mult))
            dv(lambda: V.tensor_tensor(out=t3[:], in0=t1[:], in1=t2[:], op=ALU.subtract))
            dv(lambda: V.tensor_tensor(out=fi[:], in0=t3[:], in1=den[:], op=ALU.mult))
            bbr = T("bbr", (128, 64, 16)); bbi = T("bbi", (128, 64, 16))
            ta = T("ta", (128, 64, 16)); tb = T("tb", (128, 64, 16))

            def bc(ap2):
                return ap2.unsqueeze(2).to_broadcast([128, 64, 16])

            cmul(bbr[:], bbi[:], bc(fr[:]), bc(fi[:]), bre[:], bim[:], ta[:], tb[:])
            GC = 16
            EZr = T("EZr", (128, GC, 8, 16)); EZi = T("EZi", (128, GC, 8, 16))
            FZr = T("FZr", (128, GC, 8, 16)); FZi = T("FZi", (128, GC, 8, 16))
            WCr = T("WCr", (128, GC, 8, 16)); WCi = T("WCi", (128, GC, 8, 16))
            ta2 = T("ta2", (128, GC, 16)); tb2 = T("tb2", (128, GC, 16))
            wz_t = [sb2(f"wz_t{i}", [128, 2, 128], BF16) for i in range(2)]
            wy_t = [sb2(f"wy_t{i}", [128, 128], BF16) for i in range(2)]
            wy_f = sb2("wy_f", [128, 128])
            wc_t = [sb2(f"wc_t{i}", [128, 2, 128], BF16) for i in range(2)]
            wz_tb = [Buf(), Buf()]; wy_tb = [Buf(), Buf()]; wc_tb = [Buf(), Buf()]; wy_fb = Buf()
            for i in range(2):
                op("pool", lambda i=i: nc.gpsimd.memset(wz_t[i][:], 0.0), writes=[wz_tb[i]])
                op("pool", lambda i=i: nc.gpsimd.memset(wc_t[i][:], 0.0), writes=[wc_tb[i]])
            flat = lambda a: a.rearrange("p t h -> p (t h)")

            def bcg(ap2, g0):
                return ap2[:, g0:g0 + GC].unsqueeze(2).to_broadcast([128, GC, 16])

            for g0 in range(0, 64, GC):
                gs = slice(g0, g0 + GC)
                for t in range(8):
                    cmul(EZr[:, :, t, :], EZi[:, :, t, :], bcg(Pr[:, 7 - t, :], g0), bcg(Pi[:, 7 - t, :], g0), bbr[:, gs, :], bbi[:, gs, :], ta2[:], tb2[:])
                    cmul(FZr[:, :, t, :], FZi[:, :, t, :], bcg(Gr[:, 7 - t, :], g0), bcg(Gi[:, 7 - t, :], g0), cre[:, gs, :], cim[:, gs, :], ta2[:], tb2[:])
                    cmul(WCr[:, :, t, :], WCi[:, :, t, :], bcg(Pr[:, t + 1, :], g0), bcg(Pi[:, t + 1, :], g0), cre[:, gs, :], cim[:, gs, :], ta2[:], tb2[:])
                dv(lambda: V.tensor_scalar(out=FZi[:], in0=FZi[:], scalar1=-1.0, scalar2=None, op0=ALU.mult))
                dv(lambda: V.tensor_scalar(out=WCi[:], in0=WCi[:], scalar1=-1.0, scalar2=None, op0=ALU.mult))
                for gl in range(GC):
                    g = g0 + gl
                    i = g % 2
                    gi = g % 2
                    c0 = gi * 64
                    pa = nxt("pm")
                    op("pe", lambda: nc.tensor.transpose(out=bank[pa][:, 0:128], in_=flat(EZr[:, gl, :, :]), identity=identf[:]),
                       reads=[pb, cb], writes=[bank_b[pa]], signal=False)
                    op("pe", lambda: nc.tensor.transpose(out=bank[pa][:, 128:256], in_=flat(EZi[:, gl, :, :]), identity=identf[:]),
                       reads=[pb, cb], writes=[bank_b[pa]], part=True)
                    op("act", lambda: nc.scalar.copy(out=wz_t[i][:, :, c0:c0 + 64],
                                                     in_=bank[pa][:, 0:256].rearrange("p (r c) -> p r c", r=2)[:, :, c0:c0 + 64]),
                       reads=[bank_b[pa]], writes=[wz_tb[i]], part=True)
                    dma("sp", wz_d[g].rearrange("p (r c) -> p r c", r=2), wz_t[i][:], dsem(f"wzs{i}"), reads=[wz_tb[i]], writes=[wz_db], part=True)
                    pk = nxt("pm")
                    op("pe", lambda: nc.tensor.matmul(bank[pk][:, 0:128], lhsT=flat(EZr[0:64, gl, :, :]), rhs=flat(FZr[0:64, gl, :, :]),
                                                      start=True, stop=False), reads=[pb], writes=[bank_b[pk]], signal=False)
                    op("pe", lambda: nc.tensor.matmul(bank[pk][:, 0:128], lhsT=flat(EZi[0:64, gl, :, :]), rhs=flat(FZi[0:64, gl, :, :]),
                                                      start=False, stop=True), reads=[pb], writes=[bank_b[pk]], part=True)
                    op("dve", lambda: V.tensor_tensor(out=wy_f[:], in0=bank[pk][:, 0:128], in1=tmask[:], op=ALU.mult),
                       reads=[bank_b[pk], pb], writes=[wy_fb])
                    op("dve", lambda: V.scalar_tensor_tensor(out=wy_t[i][:], in0=identf[:], scalar=dr[:, g:g + 1], in1=wy_f[:],
                                                             op0=ALU.mult, op1=ALU.add), reads=[wy_fb, pb, cb], writes=[wy_tb[i]])
                    dma("sp", wy_d[g], wy_t[i][:], dsem(f"wys{i}"), reads=[wy_tb[i]], writes=[wy_db], part=True)
                    op("pool", lambda: nc.gpsimd.tensor_copy(out=wc_t[i][c0:c0 + 64, 0, :], in_=flat(WCr[c0:c0 + 64, gl, :, :])),
                       reads=[pb], writes=[wc_tb[i]], part=True)
                    op("pool", lambda: nc.gpsimd.tensor_copy(out=wc_t[i][c0:c0 + 64, 1, :], in_=flat(WCi[c0:c0 + 64, gl, :, :])),
                       reads=[pb], writes=[wc_tb[i]], part=True)
                    dma("sp", wc_d[g].rearrange("p (r c) -> p r c", r=2), wc_t[i][:], dsem(f"wcs{i}"), reads=[wc_tb[i]], writes=[wc_db], part=True)
                op("dve", lambda: V.memset(t3[:, 0:1], 0.0), reads=[pb, wz_tb[0], wz_tb[1], wy_fb, wc_tb[0], wc_tb[1]] + [bank_b[b_] for b_ in PM],
                   writes=[pb], part=True)

            mag8 = T("mag8"); rm8 = T("rm8"); cth = T("cth"); sth = T("sth")
            dv(lambda: V.tensor_tensor(out=t1[:], in0=mag[:], in1=mag[:], op=ALU.mult))
            dv(lambda: V.tensor_tensor(out=t2[:], in0=t1[:], in1=t1[:], op=ALU.mult))
            dv(lambda: V.tensor_tensor(out=mag8[:], in0=t2[:], in1=t2[:], op=ALU.mult))
            dv(lambda: V.reciprocal(out=rm8[:], in_=mag8[:]))
            dv(lambda: V.tensor_tensor(out=cth[:], in0=Pr[:, 8, :], in1=rm8[:], op=ALU.mult))
            dv(lambda: V.tensor_tensor(out=sth[:], in0=Pi[:, 8, :], in1=rm8[:], op=ALU.mult))
            ck = T("ck", (128, 8, 32)); sk = T("sk", (128, 8, 32))

            def to_pair(dst, src, bufs_w=None):
                s2 = src.rearrange("p (q two) -> p q two", two=2)
                if bufs_w is None:
                    dv(lambda: V.tensor_copy(out=dst[0:64, :], in_=s2[0:64, :, 0]))
                    dv(lambda: V.tensor_copy(out=dst[64:128, :], in_=s2[64:128, :, 1]))
                else:
                    op("dve", lambda: V.tensor_copy(out=dst[0:64, :], in_=s2[0:64, :, 0]), reads=[pb], writes=bufs_w, part=True)
                    op("dve", lambda: V.tensor_copy(out=dst[64:128, :], in_=s2[64:128, :, 1]), reads=[pb], writes=bufs_w, part=True)

            to_pair(R_pair[:], mag8[:], [rp_b])
            to_pair(ck[:, 0, :], cth[:])
            to_pair(sk[:, 0, :], sth[:])
            u1 = T("u1", (128, 32)); u2 = T("u2", (128, 32))
            for k in range(1, 8):
                dv(lambda: V.tensor_tensor(out=u1[:], in0=ck[:, k - 1, :], in1=ck[:, k - 1, :], op=ALU.mult))
                dv(lambda: V.tensor_tensor(out=u2[:], in0=sk[:, k - 1, :], in1=sk[:, k - 1, :], op=ALU.mult))
                dv(lambda: V.scalar_tensor_tensor(out=sk[:, k, :], in0=ck[:, k - 1, :], scalar=2.0, in1=sk[:, k - 1, :],
                                                  op0=ALU.mult, op1=ALU.mult))
                dv(lambda: V.tensor_tensor(out=ck[:, k, :], in0=u1[:], in1=u2[:], op=ALU.subtract))
            PC = 16
            TC = T("TC", (128, PC, 256)); TS = T("TS", (128, PC, 256))
            v1 = T("v1", (128, PC, 128)); v2 = T("v2", (128, PC, 128))
            cs_v = cs_d.rearrange("q p (r n) -> p q r n", r=2)
            for p0 in range(0, 32, PC):
                dv(lambda: V.memset(TC[:, :, 0:1], 1.0)); dv(lambda: V.memset(TS[:, :, 0:1], 0.0))
                for k in range(8):
                    m = 1 << k
                    cb_ = ck[:, k, p0:p0 + PC].unsqueeze(2).to_broadcast([128, PC, m])
                    sb_ = sk[:, k, p0:p0 + PC].unsqueeze(2).to_broadcast([128, PC, m])
                    dv(lambda: V.tensor_tensor(out=v1[:, :, 0:m], in0=TC[:, :, 0:m], in1=cb_, op=ALU.mult))
                    dv(lambda: V.tensor_tensor(out=v2[:, :, 0:m], in0=TS[:, :, 0:m], in1=sb_, op=ALU.mult))
                    dv(lambda: V.tensor_tensor(out=TC[:, :, m:2 * m], in0=v1[:, :, 0:m], in1=v2[:, :, 0:m], op=ALU.subtract))
                    dv(lambda: V.tensor_tensor(out=v1[:, :, 0:m], in0=TC[:, :, 0:m], in1=sb_, op=ALU.mult))
                    dv(lambda: V.tensor_tensor(out=v2[:, :, 0:m], in0=TS[:, :, 0:m], in1=cb_, op=ALU.mult))
                    dv(lambda: V.tensor_tensor(out=TS[:, :, m:2 * m], in0=v1[:, :, 0:m], in1=v2[:, :, 0:m], op=ALU.add))
                dma("sp", cs_v[:, p0:p0 + PC, 0, :], TC[:], psem, reads=[pb], writes=[cs_db, pb], part=True)
                dma("sp", cs_v[:, p0:p0 + PC, 1, :], TS[:], psem, reads=[pb], writes=[cs_db, pb], part=True)
                if p0 == 0:
                    dbg_dump("dbg_TC", [128, PC, 256], TC[:], [pb])
                    dbg_dump("dbg_TS", [128, PC, 256], TS[:], [pb])
            dbg_dump("dbg_P8r", [128, 64], Pr[:, 8, :], [pb])
            dbg_dump("dbg_P8i", [128, 64], Pi[:, 8, :], [pb])
            kb.barrier()
        if stop_after == "prep":
            fin = {sm: sm.cnt for sm in kb.allsems if sm.cnt > 0}
            kb.wait("sp", fin)
            return nc, dbg_out

        out_b = [[Buf() for _ in range(NT)] for _ in range(NB)]
        x_b = Buf()
        attn_db = [Buf() for _ in range(8)]
        ssm_db = [Buf() for _ in range(8)]

        def norm_transpose(sbp, sfx, src_of_tt, srcbuf_of_tt, hT, hT_b, nslot):
            xin = [sbp(f"xin{sfx}{i}", [128, D]) for i in range(nslot)]
            xin_b = [Buf() for _ in range(nslot)]
            xnb = [sbp(f"xnb{sfx}{i}", [128, D], BF16) for i in range(nslot)]
            xnb_b = [Buf() for _ in range(nslot)]
            stat = sbp(f"stat{sfx}", [128, 8])
            stat_b = Buf()
            for tt in range(NT):
                i = tt % nslot
                dma("sp", xin[i][:], src_of_tt(tt), dsem(f"xin{i}"), reads=[srcbuf_of_tt(tt)], writes=[xin_b[i]])
                op("act", lambda: nc.scalar.activation(out=xnb[i][:], in_=xin[i][:], func=AF.Square, accum_out=stat[:, 0:1]),
                   reads=[xin_b[i]], writes=[xnb_b[i], stat_b])
                op("act", lambda: nc.scalar.activation(out=stat[:, 1:2], in_=stat[:, 0:1], func=AF.Ln, scale=1.0 / D, bias=epst[:]),
                   reads=[stat_b, cb], writes=[stat_b])
                op("act", lambda: nc.scalar.activation(out=stat[:, 2:3], in_=stat[:, 1:2], func=AF.Exp, scale=-0.5),
                   reads=[stat_b], writes=[stat_b])
                op("dve", lambda: V.tensor_scalar(out=xnb[i][:], in0=xin[i][:], scalar1=stat[:, 2:3], scalar2=None, op0=ALU.mult),
                   reads=[xin_b[i], stat_b], writes=[xnb_b[i]])
                for half in range(2):
                    for k in range(8):
                        kt = half * 8 + k
                        op("pe", lambda: nc.tensor.transpose(out=ptr[:, k, :], in_=xnb[i][:, kt * 128:(kt + 1) * 128], identity=identb[:]),
                           reads=[xnb_b[i], cb], writes=[ptr_b], signal=(k == 7), part=(k > 0))
                    dstv = hT[:, half * 8:half * 8 + 8, tt * 128:(tt + 1) * 128]
                    if half == 0:
                        op("act", lambda: nc.scalar.copy(out=dstv, in_=ptr), reads=[ptr_b], writes=[hT_b[tt]], part=False)
                    else:
                        op("dve", lambda: V.tensor_copy(out=dstv, in_=ptr), reads=[ptr_b], writes=[hT_b[tt]], part=True)

        def phase_m1(s):
            with contextlib.ExitStack() as esp:
                def sbp(name, shape, dt=F32):
                    return esp.enter_context(nc.sbuf_tensor(f"{name}_s{s}", list(shape), dt))

                hT = sbp("hT", [128, KT, L], BF16)
                hT_b = [Buf() for _ in range(NT)]
                norm_transpose(sbp, "m", lambda tt: x[s, tt * 128:(tt + 1) * 128, :], lambda tt: x_b, hT, hT_b, 1)
                if stop_after == "m1_norm":
                    dbg_dump("dbg_hT", [128, KT, L], hT[:], hT_b, BF16)
                    raise _Stop()

                NWS = 3
                wst = [sbp(f"wst{i}", [128, KT, 128], BF16) for i in range(NWS)]
                wst_b = [Buf() for _ in range(NWS)]
                wrr = [0]

                def load_w_in(ft):
                    i = wrr[0] % NWS
                    wrr[0] += 1
                    dma("sp", wst[i][:], w_in_t[ft], dsem(f"wstm{i}"), reads=[w_in_bb], writes=[wst_b[i]])
                    return i

                def proj_fm(wi, tg, pbank):
                    for kt in range(KT):
                        op("pe", lambda: nc.tensor.matmul(bank[pbank][:], lhsT=wst[wi][:, kt, :], rhs=hT[:, kt, tg * 512:(tg + 1) * 512],
                                                          start=(kt == 0), stop=(kt == KT - 1)),
                           reads=[wst_b[wi]] + hT_b[tg * 4:tg * 4 + 4], writes=[bank_b[pbank]], signal=(kt == KT - 1), part=(kt > 0))

                sq = [sbp(f"sq{i}", [128, 512], BF16) for i in range(2)]
                sq_b = [Buf() for _ in range(2)]
                qraw = [sbp(f"qraw{i}", [128, 512]) for i in range(2)]
                qraw_b = [Buf() for _ in range(2)]
                rstd = [sbp(f"rstd{i}", [128, 512]) for i in range(2)]
                rstd_b = [Buf() for _ in range(2)]
                qn = sbp("qn", [128, L], BF16)
                qn_b = [Buf() for _ in range(4)]
                knA = sbp("knA", [128, L], BF16)
                knB = sbp("knB", [128, L], BF16)
                knA_b = [Buf() for _ in range(4)]
                knB_b = [Buf() for _ in range(4)]
                ("memsets" in _SKIP) or op("dve", lambda: V.memset(knA[64:128, :], 0.0), writes=knA_b, part=True)
                ("memsets" in _SKIP) or op("dve", lambda: V.memset(knB[0:64, :], 0.0), writes=knB_b, part=True)
                vaug = sbp("vaug", [128, NT, 2, 65], BF16)
                vaug_b = Buf()
                ("memsets" in _SKIP) or op("dve", lambda: V.memset(vaug[:, :, :, 64:65], 1.0), writes=[vaug_b], part=True)
                qkc = [0]

                def qk_norm(wi, is_q):
                    if "qk_all" in _SKIP:
                        return
                    for tg in range(4):
                        pb_ = nxt("pp")
                        proj_fm(wi, tg, pb_)
                        j = qkc[0] % 2
                        qkc[0] += 1
                        lvl = min([int(t[2:]) for t in _SKIP if t.startswith("qk") and t[2:].isdigit()] + [99])
                        op("dve", lambda: V.tensor_copy(out=qraw[j][:], in_=bank[pb_][:]), reads=[bank_b[pb_]], writes=[qraw_b[j]])
                        if lvl < 1:
                            continue
                        op("act", lambda: nc.scalar.activation(out=sq[j][:], in_=qraw[j][:], func=AF.Square),
                           reads=[qraw_b[j]], writes=[sq_b[j]])
                        if lvl < 2:
                            continue
                        pmi = nxt("pm")
                        op("pe", lambda: nc.tensor.matmul(bank[pmi][:], lhsT=blk[:], rhs=sq[j][:], start=True, stop=True),
                           reads=[sq_b[j], cb], writes=[bank_b[pmi]])
                        if lvl < 3:
                            continue
                        op("act", lambda: nc.scalar.activation(out=rstd[j][:], in_=bank[pmi][:], func=AF.Ln, scale=1.0 / DH, bias=epst[:]),
                           reads=[bank_b[pmi], cb], writes=[rstd_b[j]])
                        op("act", lambda: nc.scalar.activation(out=rstd[j][:], in_=rstd[j][:], func=AF.Exp, scale=-0.5),
                           reads=[rstd_b[j]], writes=[rstd_b[j]])
                        if lvl < 4:
                            continue
                        cs = slice(tg * 512, (tg + 1) * 512)
                        if is_q:
                            op("dve", lambda: V.scalar_tensor_tensor(out=qn[:, cs], in0=qraw[j][:], scalar=gqks[:, 0:1], in1=rstd[j][:],
                                                                     op0=ALU.mult, op1=ALU.mult),
                               reads=[qraw_b[j], rstd_b[j], cb], writes=[qn_b[tg]])
                        else:
                            op("dve", lambda: V.scalar_tensor_tensor(out=knA[0:64, cs], in0=qraw[j][0:64, :], scalar=gqks[0:64, 1:2],
                                                                     in1=rstd[j][0:64, :], op0=ALU.mult, op1=ALU.mult),
                               reads=[qraw_b[j], rstd_b[j], cb], writes=[knA_b[tg]])
                            op("dve", lambda: V.scalar_tensor_tensor(out=knB[64:128, cs], in0=qraw[j][64:128, :],
                                                                     scalar=gqks[64:128, 1:2], in1=rstd[j][64:128, :],
                                                                     op0=ALU.mult, op1=ALU.mult),
                               reads=[qraw_b[j], rstd_b[j], cb], writes=[knB_b[tg]])

                def v_proj(wi):
                    for t4 in range(4):
                        pb_ = nxt("pp")
                        for q in range(4):
                            tt = t4 * 4 + q
                            for kt in range(KT):
                                last = (q == 3 and kt == KT - 1)
                                op("pe", lambda: nc.tensor.matmul(bank[pb_][:, q * 128:(q + 1) * 128], lhsT=hT[:, kt, tt * 128:(tt + 1) * 128],
                                                                  rhs=wst[wi][:, kt, :], start=(q == 0 and kt == 0), stop=last,
                                                                  skip_group_check=True),
                                   reads=[wst_b[wi], hT_b[tt]], writes=[bank_b[pb_]], signal=last, part=not (q == 0 and kt == 0))
                        op("act", lambda: nc.scalar.copy(out=vaug[:, t4 * 4:t4 * 4 + 4, :, 0:64],
                                                         in_=bank[pb_][:].rearrange("p (q h d) -> p q h d", q=4, h=2)),
                           reads=[bank_b[pb_]], writes=[vaug_b], part=True)

                kaug = sbp("kaug", [128, L], BF16)
                qaug = sbp("qaug", [128, L], BF16)
                kaug_b, qaug_b = Buf(), Buf()
                ("memsets" in _SKIP) or op("dve", lambda: V.memset(kaug[:], 0.0), writes=[kaug_b])
                ("memsets" in _SKIP) or op("dve", lambda: V.memset(qaug[:], 0.0), writes=[qaug_b])
                kmean = sbp("kmean", [128, 8])
                kmh = sbp("kmh", [128, 16], BF16)
                kmt = sbp("kmt", [128, 8])
                km_b = Buf()
                gm = sbp("gm", [128, 8]); top8 = sbp("top8", [128, 8]); gsum = sbp("gsum", [128, 8, 8])
                seln = sbp("seln", [128, 8, 8], BF16)
                sel_b = Buf()
                pT = [sbp(f"pT{i}", [128, 512], BF16) for i in range(3)]
                pT_b = [Buf() for _ in range(3)]
                ptc = [0]
                sdg = sbp("sdg", [128, 512])
                sdg_b = Buf()
                rden = sbp("rden", [128, 4])
                rden_b = Buf()
                atm = sbp("atm", [128, NT, 128], BF16)
                atm_b = Buf()
                ssqa = sbp("ssqa", [128, NT, 8])
                ssqa_b = Buf()
                junk = sbp("junk", [128, 128])
                junk_b = Buf()
                atT = sbp("atT", [128, L], BF16)
                atT_b = Buf()

                def attention_head(h):
                    hb = (h % 2) * 64
                    hs = slice(hb, hb + 64)
                    kn_t = knA if h % 2 == 0 else knB
                    kn_bs = knA_b if h % 2 == 0 else knB_b
                    dma("sp", kaug[0:NA, :], c_kaug[h], dsem("augk"), writes=[kaug_b], part=True)
                    dma("sp", qaug[0:NA, :], c_qaug[h], dsem("augq"), writes=[qaug_b], part=True)
                    op("dve", lambda: V.tensor_reduce(out=kmean[hs, :], in_=kn_t[hs, :].rearrange("p (b k) -> p b k", b=8),
                                                      axis=mybir.AxisListType.X, op=ALU.add), reads=kn_bs, writes=[km_b])
                    op("dve", lambda: V.tensor_scalar(out=kmean[hs, :], in0=kmean[hs, :], scalar1=1.0 / 256.0, scalar2=None,
                                                      op0=ALU.mult), reads=[km_b], writes=[km_b])
                    op("dve", lambda: V.tensor_copy(out=kmh[hs, 0:8], in_=kmean[hs, :]), reads=[km_b], writes=[km_b])
                    op("dve", lambda: V.tensor_tensor(out=kmt[hs, :], in0=kmean[hs, :], in1=kmh[hs, 0:8], op=ALU.subtract),
                       reads=[km_b], writes=[km_b])
                    op("dve", lambda: V.tensor_copy(out=kmh[hs, 8:16], in_=kmt[hs, :]), reads=[km_b], writes=[km_b])
                    pg = nxt("pm")
                    for i8 in range(8):
                        tt = 8 + i8
                        op("pe", lambda: nc.tensor.matmul(bank[pg][:, i8 * 16:(i8 + 1) * 16], lhsT=qn[hs, tt * 128:(tt + 1) * 128],
                                                          rhs=kmh[hs, :], start=(i8 == 0), stop=(i8 == 7), skip_group_check=True),
                           reads=[km_b] + qn_b, writes=[bank_b[pg]], signal=(i8 == 7), part=(i8 > 0))
                    gv = bank[pg][:, 0:128].rearrange("p (t c j) -> p t c j", t=8, c=2)
                    op("dve", lambda: V.tensor_copy(out=gsum[:], in_=gv[:, :, 0, :]), reads=[bank_b[pg]], writes=[sel_b])
                    op("dve", lambda: V.tensor_tensor(out=gsum[:], in0=gsum[:], in1=gv[:, :, 1, :], op=ALU.add),
                       reads=[bank_b[pg], sel_b], writes=[sel_b])
                    for i8 in range(8):
                        b = (8 + i8) // 2
                        op("dve", lambda: V.tensor_tensor(out=gm[:], in0=gsum[:, i8, :], in1=padm[:, b, :], op=ALU.add),
                           reads=[sel_b, cb], writes=[sel_b])
                        op("dve", lambda: V.max(out=top8[:], in_=gm[:]), reads=[sel_b], writes=[sel_b])
                        op("dve", lambda: V.scalar_tensor_tensor(out=seln[:, i8, :], in0=gm[:], scalar=top8[:, 2:3],
                                                                 in1=negm[:, b, :], op0=ALU.is_lt, op1=ALU.mult),
                           reads=[sel_b, cb], writes=[sel_b])
                    for i8 in range(8):
                        op("pe", lambda: nc.tensor.transpose(out=ptr[0:8, i8, :], in_=seln[:, i8, :], identity=identb[:]),
                           reads=[sel_b, cb], writes=[ptr_b], signal=(i8 == 7), part=(i8 > 0))
                    op("act", lambda: nc.scalar.copy(out=qaug[0:8, 1024:2048], in_=ptr[0:8, :, :].rearrange("p t q -> p (t q)")),
                       reads=[ptr_b], writes=[qaug_b], part=True)
                    for G in range(4):
                        nk = 4 * G + 4
                        first = True
                        for kt in range(nk):
                            si = nxt("pst")
                            op("pe", lambda: nc.tensor.matmul(bank[si][:], lhsT=kn_t[:, kt * 128:(kt + 1) * 128],
                                                              rhs=qn[:, G * 512:(G + 1) * 512], start=True, stop=False),
                               reads=kn_bs + qn_b, writes=[bank_b[si]], signal=False)
                            op("pe", lambda: nc.tensor.matmul(bank[si][:], lhsT=kaug[:, kt * 128:(kt + 1) * 128],
                                                              rhs=qaug[:, G * 512:(G + 1) * 512], start=False, stop=True),
                               reads=[kaug_b, qaug_b], writes=[bank_b[si]], part=True)
                            pi_ = ptc[0] % 3
                            ptc[0] += 1
                            o = kt - 4 * G
                            if o >= 0:
                                op("dve", lambda: V.scalar_tensor_tensor(out=sdg[:], in0=bank[si][:], scalar=40.0, in1=cmask[:, o, :],
                                                                         op0=ALU.min, op1=ALU.add),
                                   reads=[bank_b[si], cb], writes=[sdg_b])
                                op("act", lambda: nc.scalar.activation(out=pT[pi_][:], in_=sdg[:], func=AF.Exp),
                                   reads=[sdg_b], writes=[pT_b[pi_]])
                            else:
                                op("act", lambda: nc.scalar.activation(out=pT[pi_][:], in_=bank[si][:], func=AF.Exp),
                                   reads=[bank_b[si]], writes=[pT_b[pi_]])
                            for qs in range(4):
                                if kt > 4 * G + qs:
                                    continue
                                lastmm = (kt == 4 * G + qs)
                                op("pe", lambda: nc.tensor.matmul(po[:, qs, :], lhsT=pT[pi_][:, qs * 128:(qs + 1) * 128],
                                                                  rhs=vaug[:, kt, h % 2, :], start=first, stop=lastmm,
                                                                  skip_group_check=True),
                                   reads=[pT_b[pi_], vaug_b], writes=[po_b], signal=(qs == 3 or lastmm), part=not first)
                                first = False
                        op("dve", lambda: V.reciprocal(out=rden[:], in_=po[:, :, 64]), reads=[po_b], writes=[rden_b])
                        op("dve", lambda: V.tensor_tensor(out=atm[:, G * 4:G * 4 + 4, hb:hb + 64], in0=po[:, :, 0:64],
                                                          in1=rden[:].unsqueeze(2).to_broadcast([128, 4, 64]), op=ALU.mult),
                           reads=[po_b, rden_b], writes=[atm_b], part=True)

                def attn_finish(hp):
                    for tt in range(NT):
                        op("act", lambda: nc.scalar.activation(out=junk[:], in_=atm[:, tt, :], func=AF.Square,
                                                               accum_out=ssqa[:, tt, hp:hp + 1]),
                           reads=[atm_b], writes=[junk_b, ssqa_b], part=True)
                    for half in range(2):
                        for k in range(8):
                            tt = half * 8 + k
                            op("pe", lambda: nc.tensor.transpose(out=ptr[:, k, :], in_=atm[:, tt, :], identity=identb[:]),
                               reads=[atm_b, cb], writes=[ptr_b], signal=(k == 7), part=(k > 0))
                        op("dve", lambda: V.tensor_copy(out=atT[:, half * 1024:(half + 1) * 1024], in_=ptr.rearrange("p t q -> p (t q)")),
                           reads=[ptr_b], writes=[atT_b], part=(half > 0))
                    dma("sp", attn_d[hp * 128:(hp + 1) * 128, :], atT[:], dsem("attn_d"), reads=[atT_b], writes=[attn_db[hp]])

                upyp = sbp("upyp", [128, L], BF16)
                up_b = Buf()
                xst = sbp("xst", [128, 8, 256], BF16)
                xst_b = Buf()
                u_db, y_db = Buf(), Buf()
                wzs_t = sbp("wzs_t", [128, 8, 256], BF16)
                wys_t = sbp("wys_t", [128, 8, 128], BF16)
                wcs_t = sbp("wcs_t", [128, 8, 256], BF16)
                sw_b = Buf()
                cst_t = [sbp(f"cst_t{i}", [128, 512]) for i in range(2)]
                cst_b = [Buf(), Buf()]
                rin = [sbp(f"rin{j}", [128, 256]) for j in range(2)]
                rin_b = Buf()
                wsc = [sbp(f"wsc{j}", [128, 256]) for j in range(2)]
                wsc_b = Buf()
                tmpa = sbp("tmpa", [128, 256]); tmpb = sbp("tmpb", [128, 256])
                tmp_b = Buf()
                sst = [[sbp(f"sst{i}{j}", [128, 258], BF16) for j in range(2)] for i in range(4)]
                sst_b = [Buf() for _ in range(4)]
                yall = sbp("yall", [128, 8, 256], BF16)
                yall_b = Buf()
                ga = sbp("ga", [128, 512]); gb = sbp("gb", [128, 512]); gc = sbp("gc", [128, 512])
                ga_b, gb_b, gc_b = Buf(), Buf(), Buf()
                smo = sbp("smo", [128, L], BF16)
                smo_b = Buf()
                for i in range(4):
                    for j in range(2):
                        ("memsets" in _SKIP) or op("dve", lambda i=i, j=j: V.memset(sst[i][j][:, 0:2], 0.0), writes=[sst_b[i]], part=True)
                csc = [0]

                def ssm_tile(ft):
                    wi = load_w_in(24 + ft)
                    dma("sp", wzs_t[:], wz_d[ft * 8:(ft + 1) * 8].rearrange("g p c -> p g c"), dsem("sw"), reads=[wz_db], writes=[sw_b])
                    dma("sp", wys_t[:], wy_d[ft * 8:(ft + 1) * 8].rearrange("g p c -> p g c"), dsem("sw"), reads=[wy_db], writes=[sw_b], part=True)
                    dma("sp", wcs_t[:], wc_d[ft * 8:(ft + 1) * 8].rearrange("g p c -> p g c"), dsem("sw"), reads=[wc_db], writes=[sw_b], part=True)
                    upv = upyp[:].rearrange("p (t n) -> p n t", t=8)
                    for tg in range(4):
                        pb_ = nxt("pp")
                        proj_fm(wi, tg, pb_)
                        op("act", lambda: nc.scalar.copy(out=upv[:, tg * 64:(tg + 1) * 64, :],
                                                         in_=bank[pb_][:].rearrange("p (n t) -> p n t", t=8)),
                           reads=[bank_b[pb_]], writes=[up_b], part=(tg > 0))
                    dma("sp", u_d[ft * 128:(ft + 1) * 128, :], upyp[:], dsem("u_d"), reads=[up_b], writes=[u_db])
                    src = u_d[ft * 128:(ft + 1) * 128, :].rearrange("(g h) (t n) -> t h g n", h=16, t=8)
                    for t in range(8):
                        dma("sp", xst[t * 16:(t + 1) * 16, :, :], src[t], dsem("xst"), reads=[u_db], writes=[xst_b], part=(t > 0))
                    for q in range(4):
                        pair = ft * 4 + q
                        st = sst[q]
                        ci = csc[0] % 2
                        csc[0] += 1
                        dma("sp", cst_t[ci][:], cs_d[pair], dsem(f"cs{ci}"), reads=[cs_db], writes=[cst_b[ci]])
                        pz = nxt("pm")
                        for r in range(2):
                            for gi in range(2):
                                gl = q * 2 + gi
                                f_ = (r == 0 and gi == 0)
                                l_ = (r == 1 and gi == 1)
                                op("pe", lambda: nc.tensor.matmul(bank[pz][:, r * 256:(r + 1) * 256],
                                                                  lhsT=wzs_t[:, gl, r * 128:(r + 1) * 128], rhs=xst[:, gl, :],
                                                                  start=f_, stop=l_, skip_group_check=True),
                                   reads=[sw_b, xst_b], writes=[bank_b[pz]], signal=l_, part=not f_)
                        zr = bank[pz][:, 0:256]
                        zi = bank[pz][:, 256:512]
                        C = cst_t[ci][:, 0:256]
                        S = cst_t[ci][:, 256:512]
                        rd = [bank_b[pz], cst_b[ci]]
                        op("dve", lambda: V.tensor_tensor(out=tmpa[:], in0=zr, in1=C, op=ALU.mult), reads=rd, writes=[tmp_b])
                        op("dve", lambda: V.tensor_tensor(out=tmpb[:], in0=zi, in1=S, op=ALU.mult), reads=rd, writes=[tmp_b], part=True)
                        op("dve", lambda: V.tensor_tensor(out=rin[0][:], in0=tmpa[:], in1=tmpb[:], op=ALU.add), reads=[tmp_b], writes=[rin_b])
                        op("dve", lambda: V.tensor_tensor(out=tmpa[:], in0=zi, in1=C, op=ALU.mult), reads=rd, writes=[tmp_b])
                        op("dve", lambda: V.tensor_tensor(out=tmpb[:], in0=zr, in1=S, op=ALU.mult), reads=rd, writes=[tmp_b], part=True)
                        op("dve", lambda: V.tensor_tensor(out=rin[1][:], in0=tmpa[:], in1=tmpb[:], op=ALU.subtract), reads=[tmp_b],
                           writes=[rin_b], part=True)
                        Rb = R_pair[:, pair:pair + 1].to_broadcast([128, 256])
                        for r in range(2):
                            op("dve", lambda: V.tensor_tensor_scan(out=wsc[r][:], data0=Rb, data1=rin[r][:], initial=0.0,
                                                                   op0=ALU.mult, op1=ALU.add),
                               reads=[rin_b, rp_b], writes=[wsc_b], part=(r > 0))
                        rw = [wsc_b, cst_b[ci]]
                        op("dve", lambda: V.tensor_tensor(out=tmpa[:], in0=wsc[0][:], in1=C, op=ALU.mult), reads=rw, writes=[tmp_b])
                        op("dve", lambda: V.tensor_tensor(out=tmpb[:], in0=wsc[1][:], in1=S, op=ALU.mult), reads=rw, writes=[tmp_b], part=True)
                        op("dve", lambda: V.tensor_tensor(out=st[0][:, 2:258], in0=tmpa[:], in1=tmpb[:], op=ALU.subtract),
                           reads=[tmp_b], writes=[sst_b[q]], part=True)
                        op("dve", lambda: V.tensor_tensor(out=tmpa[:], in0=wsc[1][:], in1=C, op=ALU.mult), reads=rw, writes=[tmp_b])
                        op("dve", lambda: V.tensor_tensor(out=tmpb[:], in0=wsc[0][:], in1=S, op=ALU.mult), reads=rw, writes=[tmp_b], part=True)
                        op("dve", lambda: V.tensor_tensor(out=st[1][:, 2:258], in0=tmpa[:], in1=tmpb[:], op=ALU.add),
                           reads=[tmp_b], writes=[sst_b[q]], part=True)
                    for q in range(4):
                        st = sst[q]
                        py = nxt("pm")
                        for gi in range(2):
                            gl = q * 2 + gi
                            oc = slice(gi * 256, gi * 256 + 256)
                            op("pe", lambda: nc.tensor.matmul(bank[py][:, oc], lhsT=wys_t[:, gl, :], rhs=xst[:, gl, :],
                                                              start=(gi == 0), stop=False, skip_group_check=True),
                               reads=[sw_b, xst_b], writes=[bank_b[py]], signal=False, part=(gi > 0))
                            op("pe", lambda: nc.tensor.matmul(bank[py][:, oc], lhsT=wcs_t[:, gl, 0:128], rhs=st[0][:, 1:257],
                                                              start=False, stop=False, skip_group_check=True),
                               reads=[sw_b, sst_b[q]], writes=[bank_b[py]], signal=False, part=True)
                            op("pe", lambda: nc.tensor.matmul(bank[py][:, oc], lhsT=wcs_t[:, gl, 128:256], rhs=st[1][:, 1:257],
                                                              start=False, stop=(gi == 1), skip_group_check=True),
                               reads=[sw_b, sst_b[q]], writes=[bank_b[py]], signal=(gi == 1), part=True)
                        op("act", lambda: nc.scalar.copy(out=yall[:, q * 2:q * 2 + 2, :], in_=bank[py][:].rearrange("p (g n) -> p g n", g=2)),
                           reads=[bank_b[py]], writes=[yall_b], part=(q > 0))
                    dst = y_d[ft * 128:(ft + 1) * 128, :].rearrange("(g h) (t n) -> t h g n", h=16, t=8)
                    for t in range(8):
                        dma("sp", dst[t], yall[t * 16:(t + 1) * 16, :, :], dsem("y_d"), reads=[yall_b], writes=[y_db], part=(t > 0))
                    dma("sp", upyp[:], y_d[ft * 128:(ft + 1) * 128, :], dsem("yp"), reads=[y_db], writes=[up_b])
                    ypn = upyp[:].rearrange("p (t n) -> p n t", t=8)
                    for c in range(4):
                        op("act", lambda: nc.scalar.copy(out=ga[:].rearrange("p (n t) -> p n t", t=8), in_=ypn[:, c * 64:(c + 1) * 64, :]),
                           reads=[up_b], writes=[ga_b])
                        op("dve", lambda: V.tensor_scalar(out=gc[:], in0=ga[:], scalar1=-9.0, scalar2=None, op0=ALU.max),
                           reads=[ga_b], writes=[gc_b])
                        op("pool", lambda: nc.gpsimd.tensor_tensor(out=gb[:], in0=gc[:], in1=gc[:], op=ALU.mult), reads=[gc_b], writes=[gb_b])
                        op("dve", lambda: V.tensor_scalar(out=gb[:], in0=gb[:], scalar1=0.044715, scalar2=1.0, op0=ALU.mult, op1=ALU.add),
                           reads=[gb_b], writes=[gb_b])
                        op("pool", lambda: nc.gpsimd.tensor_tensor(out=gb[:], in0=gb[:], in1=gc[:], op=ALU.mult), reads=[gc_b, gb_b], writes=[gb_b])
                        op("act", lambda: nc.scalar.activation(out=gc[:], in_=gb[:], func=AF.Exp, scale=-1.5957691216057308),
                           reads=[gb_b], writes=[gc_b])
                        op("dve", lambda: V.tensor_scalar(out=gc[:], in0=gc[:], scalar1=1.0, scalar2=None, op0=ALU.add), reads=[gc_b], writes=[gc_b])
                        op("dve", lambda: V.reciprocal(out=gc[:], in_=gc[:]), reads=[gc_b], writes=[gc_b])
                        op("dve", lambda: V.tensor_tensor(out=smo[:, c * 512:(c + 1) * 512], in0=ga[:], in1=gc[:], op=ALU.mult),
                           reads=[ga_b, gc_b], writes=[smo_b], part=(c > 0))
                    dma("sp", ssm_d[ft * 128:(ft + 1) * 128, :], smo[:], dsem("ssm_d"), reads=[smo_b], writes=[ssm_db[ft]])

                for hp in range(8):
                    wq = load_w_in(hp)
                    wk = load_w_in(8 + hp)
                    qk_norm(wq, True)
                    qk_norm(wk, False)
                    if stop_after == "m1_qk":
                        dbg_dump("dbg_qn", [128, L], qn[:], qn_b, BF16)
                        dbg_dump("dbg_knA", [128, L], knA[:], knA_b, BF16)
                        raise _Stop()
                    wv = load_w_in(16 + hp)
                    v_proj(wv)
                    for hh in range(2):
                        attention_head(hp * 2 + hh)
                    attn_finish(hp)
                    if stop_after == "m1_attn":
                        dbg_dump("dbg_qn", [128, L], qn[:], qn_b, BF16)
                        dbg_dump("dbg_knA", [128, L], knA[:], knA_b, BF16)
                        dbg_dump("dbg_atm", [128, NT, 128], atm[:], [atm_b], BF16)
                        raise _Stop()
                    if s == 0 and hp == 0:
                        dbg_dump("dbg_qn", [128, L], qn[:], qn_b, BF16)
                        dbg_dump("dbg_knA", [128, L], knA[:], knA_b, BF16)
                        dbg_dump("dbg_atm", [128, NT, 128], atm[:], [atm_b], BF16)
                    ssm_tile(hp)
                    if stop_after == "m1_ssm":
                        dbg_dump("dbg_qn", [128, L], qn[:], qn_b, BF16)
                        dbg_dump("dbg_knA", [128, L], knA[:], knA_b, BF16)
                        dbg_dump("dbg_atm", [128, NT, 128], atm[:], [atm_b], BF16)
                        dbg_dump("dbg_yp", [128, L], upyp[:], [up_b], BF16)
                        dbg_dump("dbg_smo", [128, L], smo[:], [smo_b], BF16)
                        raise _Stop()
                    if s == 0 and hp == 0:
                        dbg_dump("dbg_yp", [128, L], upyp[:], [up_b], BF16)
                        dbg_dump("dbg_smo", [128, L], smo[:], [smo_b], BF16)
                    if s == 0:
                        wprep_some(3)
                if s == 0:
                    wprep_some(100000)
                op("dve", lambda: V.tensor_reduce(out=rs_a[:], in_=ssqa[:], axis=mybir.AxisListType.X, op=ALU.add),
                   reads=[ssqa_b], writes=[rsa_b])
                op("act", lambda: nc.scalar.activation(out=rs_a[:], in_=rs_a[:], func=AF.Ln, scale=1.0 / 1024.0, bias=epst[:]),
                   reads=[rsa_b, cb], writes=[rsa_b])
                op("act", lambda: nc.scalar.activation(out=rs_a[:], in_=rs_a[:], func=AF.Exp, scale=-0.5), reads=[rsa_b], writes=[rsa_b])
                kb.barrier()

        rs_a = sb("rs_a", [128, NT]); rs_s = sb("rs_s", [128, NT])
        rsa_b, rss_b = Buf(), Buf()

        def phase_m2(s):
            with contextlib.ExitStack() as esp:
                def sbp(name, shape, dt=F32):
                    return esp.enter_context(nc.sbuf_tensor(f"{name}_s{s}", list(shape), dt))

                attnT = sbp("attnT", [128, 8, L], BF16)
                ssmT = sbp("ssmT", [128, 8, L], BF16)
                attnT_b = [Buf() for _ in range(8)]
                ssmT_b = [Buf() for _ in range(8)]
                for c in range(8):
                    dma("sp", ssmT[:, c, :], ssm_d[c * 128:(c + 1) * 128, :], dsem("ldm2"), reads=[ssm_db[c]], writes=[ssmT_b[c]])
                for c in range(8):
                    dma("sp", attnT[:, c, :], attn_d[c * 128:(c + 1) * 128, :], dsem("ldm2"), reads=[attn_db[c]], writes=[attnT_b[c]])
                wgl = sbp("wgl", [128, 8, 1024], BF16)
                wgl_b = Buf()
                gtmp = sbp("gtmp", [128, 8, 512], BF16)
                gtmp_b = Buf()
                gsg = sbp("gsg", [128, 512]); gsg_b = Buf()
                sqg = sbp("sqg", [128, 8, 512], BF16); sqg_b = Buf()
                dma("sp", wgl[:], w_glu_b.rearrange("(ct p) e -> p ct e", p=128), dsem("wgl"), reads=[w_glu_bb], writes=[wgl_b])
                for tg in range(4):
                    for et in range(8):
                        pb_ = nxt("pp")
                        for ct in range(8):
                            op("pe", lambda: nc.tensor.matmul(bank[pb_][:], lhsT=wgl[:, ct, et * 128:(et + 1) * 128],
                                                              rhs=ssmT[:, ct, tg * 512:(tg + 1) * 512], start=(ct == 0), stop=(ct == 7)),
                               reads=[wgl_b] + ssmT_b, writes=[bank_b[pb_]], signal=(ct == 7), part=(ct > 0))
                        op("act", lambda: nc.scalar.activation(out=gsg[:], in_=bank[pb_][:], func=AF.Exp, scale=-1.0,
                                                               bias=nbglus[:, et:et + 1]), reads=[bank_b[pb_], cb], writes=[gsg_b])
                        op("dve", lambda: V.tensor_scalar(out=gsg[:], in0=gsg[:], scalar1=1.0, scalar2=None, op0=ALU.add),
                           reads=[gsg_b], writes=[gsg_b])
                        op("dve", lambda: V.reciprocal(out=gsg[:], in_=gsg[:]), reads=[gsg_b], writes=[gsg_b])
                        op("dve", lambda: V.tensor_tensor(out=gtmp[:, et, :], in0=ssmT[:, et, tg * 512:(tg + 1) * 512], in1=gsg[:], op=ALU.mult),
                           reads=[gsg_b, ssmT_b[et]], writes=[gtmp_b], part=(et > 0))
                    op("pool", lambda: nc.gpsimd.tensor_tensor(out=sqg[:], in0=gtmp[:], in1=gtmp[:], op=ALU.mult),
                       reads=[gtmp_b], writes=[sqg_b])
                    for et in range(8):
                        op("pool", lambda: nc.gpsimd.tensor_copy(out=ssmT[:, et, tg * 512:(tg + 1) * 512], in_=gtmp[:, et, :]),
                           reads=[gtmp_b], writes=[ssmT_b[et]], part=True)
                    pmi = nxt("pm")
                    for q in range(4):
                        for et in range(8):
                            f_ = (q == 0 and et == 0)
                            l_ = (q == 3 and et == 7)
                            op("pe", lambda: nc.tensor.matmul(bank[pmi][:, q * 2:q * 2 + 2], lhsT=sqg[:, et, q * 128:(q + 1) * 128],
                                                              rhs=onesb[:], start=f_, stop=l_, skip_group_check=True),
                               reads=[sqg_b, cb], writes=[bank_b[pmi]], signal=l_, part=not f_)
                    op("act", lambda: nc.scalar.activation(out=rs_s[:, tg * 4:tg * 4 + 4],
                                                           in_=bank[pmi][:, 0:8].rearrange("p (q two) -> p q two", two=2)[:, :, 0],
                                                           func=AF.Ln, scale=1.0 / 1024.0, bias=epst[:]),
                       reads=[bank_b[pmi], cb], writes=[rss_b], part=(tg > 0))
                op("act", lambda: nc.scalar.activation(out=rs_s[:], in_=rs_s[:], func=AF.Exp, scale=-0.5), reads=[rss_b], writes=[rss_b])

                wo = [sbp(f"wo{i}", [128, KT, 512], BF16) for i in range(2)]
                wo_b = [Buf() for _ in range(2)]
                xr = [sbp(f"xr{i}", [128, 512]) for i in range(3)]
                xr_b = [Buf() for _ in range(3)]
                xo = [sbp(f"xo{i}", [128, 512]) for i in range(3)]
                xo_b = [Buf() for _ in range(3)]
                xc = [0]
                for dg in range(4):
                    i = dg % 2
                    dma("sp", wo[i][:], w_out_t[dg], dsem(f"wo{i}"), reads=[w_out_bb], writes=[wo_b[i]])
                    for tt in range(NT):
                        j = xc[0] % 3
                        xc[0] += 1
                        dma("sp", xr[j][:], x[s, tt * 128:(tt + 1) * 128, dg * 512:(dg + 1) * 512], dsem(f"xr{j}"), writes=[xr_b[j]])
                        pa = nxt("pp")
                        for ct in range(8):
                            op("pe", lambda: nc.tensor.matmul(bank[pa][:], lhsT=attnT[:, ct, tt * 128:(tt + 1) * 128], rhs=wo[i][:, ct, :],
                                                              start=(ct == 0), stop=(ct == 7)),
                               reads=[attnT_b[ct], wo_b[i]], writes=[bank_b[pa]], signal=(ct == 7), part=(ct > 0))
                        op("dve", lambda: V.scalar_tensor_tensor(out=xo[j][:], in0=bank[pa][:], scalar=rs_a[:, tt:tt + 1], in1=xr[j][:],
                                                                 op0=ALU.mult, op1=ALU.add),
                           reads=[bank_b[pa], rsa_b, xr_b[j]], writes=[xo_b[j]])
                        pb_ = nxt("pp")
                        for ct in range(8):
                            op("pe", lambda: nc.tensor.matmul(bank[pb_][:], lhsT=ssmT[:, ct, tt * 128:(tt + 1) * 128], rhs=wo[i][:, 8 + ct, :],
                                                              start=(ct == 0), stop=(ct == 7)),
                               reads=[ssmT_b[ct], wo_b[i]], writes=[bank_b[pb_]], signal=(ct == 7), part=(ct > 0))
                        op("dve", lambda: V.scalar_tensor_tensor(out=xo[j][:], in0=bank[pb_][:], scalar=rs_s[:, tt:tt + 1], in1=xo[j][:],
                                                                 op0=ALU.mult, op1=ALU.add),
                           reads=[bank_b[pb_], rss_b, xo_b[j]], writes=[xo_b[j]])
                        dma("sp", out[s, tt * 128:(tt + 1) * 128, dg * 512:(dg + 1) * 512], xo[j][:], dsem(f"xo{j}"), reads=[xo_b[j]],
                            writes=[out_b[s][tt]], part=(dg > 0))
                kb.barrier()

        def phase_f(s):
            with contextlib.ExitStack() as esp:
                def sbp(name, shape, dt=F32):
                    return esp.enter_context(nc.sbuf_tensor(f"{name}_s{s}", list(shape), dt))

                hT = sbp("h2T", [128, KT, L], BF16)
                hT_b = [Buf() for _ in range(NT)]
                norm_transpose(sbp, "f", lambda tt: out[s, tt * 128:(tt + 1) * 128, :], lambda tt: out_b[s][tt], hT, hT_b, 1)
                hid = sbp("hid", [128, 64, 512], BF16)
                hid_b = [Buf() for _ in range(64)]
                w1 = [sbp(f"w1_{i}", [128, KT, 128], BF16) for i in range(3)]
                w1_b = [Buf() for _ in range(3)]
                w2 = [sbp(f"w2_{i}", [128, 1024], BF16) for i in range(4)]
                w2_b = [Buf() for _ in range(4)]
                rl = [sbp(f"rl{i}", [128, 512]) for i in range(2)]; rl_b = [Buf(), Buf()]
                xr = [sbp(f"fxr{i}", [128, 512]) for i in range(3)]
                xr_b = [Buf() for _ in range(3)]
                xo = [sbp(f"fxo{i}", [128, 512]) for i in range(3)]
                xo_b = [Buf() for _ in range(3)]
                w1c = [0]; w2c = [0]; xc = [0]
                for tg in range(4):
                    for f in range(64):
                        i = w1c[0] % 3
                        w1c[0] += 1
                        dma("sp", w1[i][:], w_ff1_t[f], dsem(f"w1_{i}"), reads=[w_ff1_bb], writes=[w1_b[i]])
                        pb_ = nxt("pp")
                        for kt in range(KT):
                            op("pe", lambda: nc.tensor.matmul(bank[pb_][:], lhsT=w1[i][:, kt, :], rhs=hT[:, kt, tg * 512:(tg + 1) * 512],
                                                              start=(kt == 0), stop=(kt == KT - 1)),
                               reads=[w1_b[i]] + hT_b[tg * 4:tg * 4 + 4], writes=[bank_b[pb_]], signal=(kt == KT - 1), part=(kt > 0))
                        rj = f % 2
                        op("act", lambda: nc.scalar.activation(out=rl[rj][:], in_=bank[pb_][:], func=AF.Relu), reads=[bank_b[pb_]], writes=[rl_b[rj]])
                        if rj == 0:
                            op("dve", lambda: V.tensor_tensor(out=hid[:, f, :], in0=rl[rj][:], in1=rl[rj][:], op=ALU.mult),
                               reads=[rl_b[rj]], writes=[hid_b[f]])
                        else:
                            op("pool", lambda: nc.gpsimd.tensor_tensor(out=hid[:, f, :], in0=rl[rj][:], in1=rl[rj][:], op=ALU.mult),
                               reads=[rl_b[rj]], writes=[hid_b[f]])
                    for dh in range(2):
                        for f in range(64):
                            i2 = w2c[0] % 4
                            w2c[0] += 1
                            dma("sp", w2[i2][:], w_ff2_b[f * 128:(f + 1) * 128, dh * 1024:(dh + 1) * 1024], dsem(f"w2_{i2}"),
                                reads=[w_ff2_bb], writes=[w2_b[i2]])
                            for tq in range(4):
                                for d2 in range(2):
                                    bi = tq * 2 + d2
                                    op("pe", lambda: nc.tensor.matmul(bank[bi][:], lhsT=hid[:, f, tq * 128:(tq + 1) * 128],
                                                                      rhs=w2[i2][:, d2 * 512:(d2 + 1) * 512], start=(f == 0), stop=(f == 63)),
                                       reads=[hid_b[f], w2_b[i2]], writes=[bank_b[bi]], signal=(f == 63 or (tq == 3 and d2 == 1)), part=(f > 0))
                        for tq in range(4):
                            tt = tg * 4 + tq
                            for d2 in range(2):
                                bi = tq * 2 + d2
                                dg = dh * 2 + d2
                                j = xc[0] % 3
                                xc[0] += 1
                                oap = out[s, tt * 128:(tt + 1) * 128, dg * 512:(dg + 1) * 512]
                                dma("sp", xr[j][:], oap, dsem(f"xr{j}"), reads=[out_b[s][tt]], writes=[xr_b[j]])
                                op("dve", lambda: V.tensor_tensor(out=xo[j][:], in0=bank[bi][:], in1=xr[j][:], op=ALU.add),
                                   reads=[bank_b[bi], xr_b[j]], writes=[xo_b[j]])
                                dma("sp", oap, xo[j][:], dsem(f"xo{j}"), reads=[xo_b[j], xr_b[j]], writes=[out_b[s][tt]], part=True)
                kb.barrier()

        try:
            for s in range(NB):
                phase_m1(s)
                if stop_after == "m1":
                    break
                phase_m2(s)
                if stop_after == "m2":
                    break
                phase_f(s)
                if stop_after == "f":
                    break
        except _Stop:
            pass

        fin = {sm: sm.cnt for sm in kb.allsems if sm.cnt > 0}
        kb.wait("sp", fin)
    except AssertionError:
        if stop_after is None:
            raise
    return nc, dbg_out


def _bf16(a):
    return np.asarray(a, dtype=np.float32).astype(ml_dtypes.bfloat16)


def _split3(a):
    a = np.asarray(a, dtype=np.float32)
    h = a.astype(ml_dtypes.bfloat16)
    r = a - h.astype(np.float32)
    m = r.astype(ml_dtypes.bfloat16)
    r2 = r - m.astype(np.float32)
    l = r2.astype(ml_dtypes.bfloat16)
    return h, m, l


def _constants():
    c = {}
    c["c_identf"] = np.eye(128, dtype=np.float32)
    c["c_identb"] = _bf16(np.eye(128))
    blk = np.zeros((128, 128), np.float32)
    blk[:64, :64] = 1
    blk[64:, 64:] = 1
    c["c_blk"] = _bf16(blk)
    p = np.arange(128)[:, None]
    q = np.arange(512)[None, :]
    cm = np.stack([(128 * o + p <= q) for o in range(4)], axis=1).astype(np.float32)
    c["c_cmask"] = _bf16((1.0 - cm) * NEGBIG)
    pos = np.arange(L, dtype=np.float32)
    slopes = np.exp2(-8.0 * (np.arange(H, dtype=np.float32) + 1.0) / H).astype(np.float32)
    kaug = np.zeros((H, NA, L), ml_dtypes.bfloat16)
    qaug = np.zeros((H, NA, L), ml_dtypes.bfloat16)
    blkid = (np.arange(L) // 256)
    for h in range(H):
        for j in range(8):
            kaug[h, j] = _bf16((blkid == j).astype(np.float32))
        kaug[h, 8:11] = _bf16(np.ones((3, L)))
        a3 = _split3(slopes[h] * pos)
        for i in range(3):
            kaug[h, 11 + i] = a3[i]
        b3 = _split3(-slopes[h] * pos)
        for i in range(3):
            qaug[h, 8 + i] = b3[i]
        qaug[h, 11:14] = _bf16(np.ones((3, L)))
    c["c_kaug"] = kaug
    c["c_qaug"] = qaug
    padm = np.zeros((128, 8, 8), np.float32)
    negm = np.zeros((128, 8, 8), np.float32)
    for b in range(8):
        padm[:, b, b:] = -1e30
        negm[:, b, :b] = NEGBIG
    c["c_padm"] = padm
    c["c_negm"] = negm
    t = np.arange(128) // 16
    c["c_tmask"] = (t[None, :] >= t[:, None]).astype(np.float32)
    return c


def _prep_inputs(inp):
    f = lambda a: np.ascontiguousarray(np.asarray(a, dtype=np.float32))
    shared = {}
    shared["w_in"] = f(inp["w_in"][0])
    shared["w_out"] = f(inp["w_out"][0])
    shared["w_ff1"] = f(inp["w_ff1"][0])
    shared["w_ff2"] = f(inp["w_ff2"][0])
    shared["w_glu"] = f(inp["w_glu"][0])
    shared["g1t"] = f(inp["norm1_gain"][0].reshape(16, 128).T)
    shared["g2t"] = f(inp["norm2_gain"][0].reshape(16, 128).T)
    gout = np.concatenate([np.asarray(inp["attn_out_gain"][0]), np.asarray(inp["ssm_out_gain"][0])])
    shared["goutt"] = f(gout.reshape(16, 128).T)
    qg = np.asarray(inp["q_norm_gain"][0])
    kg = np.asarray(inp["k_norm_gain"][0])
    shared["gqk"] = f(np.stack([np.tile(qg, 2), np.tile(kg, 2)], axis=1))
    shared["bglu"] = f(np.asarray(inp["b_glu"][0]).reshape(8, 128).T)
    dup = lambda a: f(np.concatenate([a, a], axis=0))
    shared["lam_re"] = dup(np.asarray(inp["lambda_re"][0]).T)
    shared["lam_im"] = dup(np.asarray(inp["lambda_im"][0]).T)
    shared["logdt"] = f(np.tile(np.asarray(inp["log_dt"][0])[None, :], (128, 1)))
    shared["b_re"] = dup(np.asarray(inp["b_re"][0]).transpose(1, 0, 2))
    shared["b_im"] = dup(np.asarray(inp["b_im"][0]).transpose(1, 0, 2))
    shared["c_re"] = dup(np.asarray(inp["c_re"][0]).transpose(2, 0, 1))
    shared["c_im"] = dup(np.asarray(inp["c_im"][0]).transpose(2, 0, 1))
    d = np.asarray(inp["d_skip"][0]).reshape(64, 16)
    shared["drep"] = f(np.tile(d.T, (8, 1)))
    shared.update(_constants())
    return shared


_CACHE = {}


def kernel(**inputs):
    x = np.asarray(inputs["x"], dtype=np.float32)
    shared = _prep_inputs(inputs)
    if "nc" not in _CACHE:
        _CACHE["nc"] = build()[0]
    nc = _CACHE["nc"]
    in_maps = []
    for c in range(NCORES):
        m = dict(shared)
        m["x"] = np.ascontiguousarray(x[c * NB:(c + 1) * NB])
        in_maps.append(m)
    res = run_bass_kernel_spmd(nc, in_maps, core_ids=list(range(NCORES)))
    return np.concatenate([np.asarray(r["out"]) for r in res.results], axis=0).astype(np.float32)
```
